# Optimizing a Trainium2 kernel written in Bass

```python
import jax
import jax.numpy as jnp
from jax import lax
import numpy as np

D_MODEL = 1024
BATCH = 1
SEQ = 16384
DEPTH = 2

GRID_W = 64
CTX_LEN = 256
N_GROUPS = 4
GROUP_W = D_MODEL // N_GROUPS
D_MIX = N_GROUPS * GROUP_W
CONV_W = 3
CHUNK = 128
CMLP_HEADS = 4
CMLP_HD = GROUP_W // CMLP_HEADS
ATT_HEADS = 4
ATT_KV_HEADS = 2
ATT_GROUP = ATT_HEADS // ATT_KV_HEADS
ATT_HD = GROUP_W // ATT_HEADS
Q_BLOCK = 128
ROPE_THETA = 10000.0
ML_HEADS = 4
ML_HD = GROUP_W // ML_HEADS
ML_CHUNK = 128
ML_FGATE_BIAS = 3.0
N_EXPERTS = 16
EXPERT_FF = 2 * D_MODEL
CAPACITY_FACTOR = 2
MOD_PARTS = 6
EPS = 1e-6

IN_LAYOUT = (
    ('conv_x', GROUP_W), ('conv_b', GROUP_W), ('conv_c', GROUP_W),
    ('cmlp_u', GROUP_W), ('cmlp_v', GROUP_W),
    ('att_q', ATT_HEADS * ATT_HD), ('att_k', ATT_KV_HEADS * ATT_HD), ('att_v', ATT_KV_HEADS * ATT_HD),
    ('ml_q', GROUP_W), ('ml_k', GROUP_W), ('ml_v', GROUP_W), ('ml_o', GROUP_W),
    ('ml_i', 2 * ML_HEADS), ('ml_f', 2 * ML_HEADS),
)
D_IN = sum(s for _, s in IN_LAYOUT)

kernel_name = 'hybrid_diffusion_prefix_block'


def rmsnorm(x, g):
    xf = x.astype(jnp.float32)
    y = xf * lax.rsqrt(jnp.mean(xf * xf, axis=-1, keepdims=True) + EPS)
    return (y * g.astype(jnp.float32)).astype(x.dtype)


def modulate(h, shift, scale):
    return h * (1 + scale) + shift


def split_in(z):
    sizes = [s for _, s in IN_LAYOUT]
    parts = jnp.split(z, np.cumsum(sizes)[:-1].tolist(), axis=-1)
    return {name: part for (name, _), part in zip(IN_LAYOUT, parts)}


def axial_rope_tables(rows):
    pos = jnp.arange(rows * GRID_W)
    row, col = pos // GRID_W, pos % GRID_W
    axis_dim = ATT_HD // 2
    inv_freq = ROPE_THETA ** (-jnp.arange(0, axis_dim, 2, dtype=jnp.float32) / axis_dim)

    def axis_angles(p):
        a = p.astype(jnp.float32)[:, None] * inv_freq[None, :]
        return jnp.concatenate([a, a], axis=-1)

    ang = jnp.concatenate([axis_angles(row), axis_angles(col)], axis=-1)
    return jnp.cos(ang), jnp.sin(ang)


def rotate_half_axial(x):
    axis_dim = ATT_HD // 2
    quarter = axis_dim // 2

    def rh(s):
        return jnp.concatenate([-s[..., quarter:], s[..., :quarter]], axis=-1)

    return jnp.concatenate([rh(x[..., :axis_dim]), rh(x[..., axis_dim:])], axis=-1)


def apply_rope(x, cos, sin):
    xf = x.astype(jnp.float32)
    return (xf * cos + rotate_half_axial(xf) * sin).astype(x.dtype)


def short_conv_mixer(xin, gate_b, gate_c, conv_w):
    y = gate_c * xin
    y = lax.conv_general_dilated(
        y, conv_w[:, None, :], window_strides=(1,), padding=[(CONV_W // 2, CONV_W // 2)],
        dimension_numbers=('NWC', 'WIO', 'NWC'), feature_group_count=GROUP_W)
    return gate_b * y


def chunk_token_mlp(u, v, norm_g, ws, bs):
    b, l, _ = v.shape
    vn = rmsnorm(v, norm_g).reshape(b, l // CHUNK, CHUNK, CMLP_HEADS, CMLP_HD)
    mixed = jnp.einsum('hts,bcshd->bcthd', ws, vn) + bs.T[None, None, :, :, None]
    return u * mixed.reshape(b, l, GROUP_W)


def attn_qkv(pq, pk, pv, q_norm_g, k_norm_g):
    b, l, _ = pq.shape
    q = rmsnorm(pq.reshape(b, l, ATT_HEADS, ATT_HD), q_norm_g).transpose(0, 2, 1, 3)
    k = rmsnorm(pk.reshape(b, l, ATT_KV_HEADS, ATT_HD), k_norm_g).transpose(0, 2, 1, 3)
    v = pv.reshape(b, l, ATT_KV_HEADS, ATT_HD).transpose(0, 2, 1, 3)
    return q, k, v


def gqa_attend(q, k, v):
    s = jnp.einsum('bkgqd,bksd->bkgqs', q, k).astype(jnp.float32) * (ATT_HD ** -0.5)
    p = jax.nn.softmax(s, axis=-1).astype(v.dtype)
    return jnp.einsum('bkgqs,bksd->bkgqd', p, v)


def context_attention(q, k, v):
    b, _, l, _ = q.shape
    o = gqa_attend(q.reshape(b, ATT_KV_HEADS, ATT_GROUP, l, ATT_HD), k, v)
    return o.transpose(0, 3, 1, 2, 4).reshape(b, l, ATT_HEADS * ATT_HD)


def latent_attention(q, k, v, k_ctx, v_ctx):
    b, _, n, _ = q.shape
    k_all = jnp.concatenate([k_ctx, k], axis=2)
    v_all = jnp.concatenate([v_ctx, v], axis=2)
    nb = n // Q_BLOCK
    qb = q.reshape(b, ATT_KV_HEADS, ATT_GROUP, nb, Q_BLOCK, ATT_HD).transpose(3, 0, 1, 2, 4, 5)
    out = lax.map(lambda qblk: gqa_attend(qblk, k_all, v_all), qb)
    return out.transpose(1, 0, 4, 2, 3, 5).reshape(b, n, ATT_HEADS * ATT_HD)


def mlstm_heads(t):
    b, l, _ = t.shape
    return t.reshape(b, l, ML_HEADS, ML_HD).transpose(0, 2, 1, 3).astype(jnp.float32)


def mlstm_inputs(pq, pk, pv, pi, pf, igate_b, fgate_b):
    q = mlstm_heads(pq)
    k = mlstm_heads(pk) * (ML_HD ** -0.5)
    v = mlstm_heads(pv)
    li = (pi.astype(jnp.float32) + igate_b.astype(jnp.float32)).transpose(0, 2, 1)
    lf = jax.nn.log_sigmoid(pf.astype(jnp.float32) + fgate_b.astype(jnp.float32)).transpose(0, 2, 1)
    fwd = (li[:, :ML_HEADS], lf[:, :ML_HEADS])
    bwd = (li[:, ML_HEADS:], lf[:, ML_HEADS:])
    return q, k, v, fwd, bwd


def zero_state(b):
    return (jnp.zeros((b, ML_HEADS, ML_HD, ML_HD), jnp.float32),
            jnp.zeros((b, ML_HEADS, ML_HD), jnp.float32),
            jnp.zeros((b, ML_HEADS), jnp.float32))


def flip_seq(t):
    return jnp.flip(t, axis=2)


def to_chunks(t):
    b, h, l = t.shape[:3]
    return t.reshape(b, h, l // ML_CHUNK, ML_CHUNK, *t.shape[3:])


def mlstm_chunk_states(k, v, li, lf, state0):
    b_cum = jnp.cumsum(lf, axis=-1)
    b_last = b_cum[..., -1]
    a = b_last[..., None] - b_cum + li
    m_loc = jnp.max(a, axis=-1)
    w = jnp.exp(a - m_loc[..., None])
    c_loc = jnp.einsum('bhcs,bhcsv,bhcsk->bhcvk', w, v, k)
    n_loc = jnp.einsum('bhcs,bhcsk->bhck', w, k)

    def step(carry, inp):
        c_prev, n_prev, m_prev = carry
        bl, ml, cl, nl = inp
        m_new = jnp.maximum(bl + m_prev, ml)
        d_old = jnp.exp(bl + m_prev - m_new)
        d_loc = jnp.exp(ml - m_new)
        c_new = d_old[..., None, None] * c_prev + d_loc[..., None, None] * cl
        n_new = d_old[..., None] * n_prev + d_loc[..., None] * nl
        return (c_new, n_new, m_new), (c_prev, n_prev, m_prev)

    xs = tuple(jnp.moveaxis(t, 2, 0) for t in (b_last, m_loc, c_loc, n_loc))
    final, prev = lax.scan(step, state0, xs)
    prev = tuple(jnp.moveaxis(t, 0, 2) for t in prev)
    return b_cum, prev, final


def mlstm_chunk_outputs(q, k, v, li, b_cum, prev):
    c_prev, n_prev, m_prev = prev
    L = q.shape[-2]
    lower = jnp.tril(jnp.ones((L, L), dtype=bool))
    dmat = b_cum[..., :, None] - b_cum[..., None, :] + li[..., None, :]
    dmat = jnp.where(lower, dmat, -jnp.inf)
    inter = b_cum + m_prev[..., None]
    m_t = jnp.maximum(inter, jnp.max(dmat, axis=-1))
    s = jnp.einsum('bhctd,bhcsd->bhcts', q, k) * jnp.exp(dmat - m_t[..., None])
    w_inter = jnp.exp(inter - m_t)
    num = jnp.einsum('bhcts,bhcsd->bhctd', s, v) + w_inter[..., None] * jnp.einsum('bhcvk,bhctk->bhctv', c_prev, q)
    den = jnp.sum(s, axis=-1) + w_inter * jnp.einsum('bhck,bhctk->bhct', n_prev, q)
    return num / jnp.maximum(jnp.abs(den), jnp.exp(-m_t))[..., None]


def mlstm_scan(q, k, v, li, lf, state0):
    qc, kc, vc, lic, lfc = (to_chunks(t) for t in (q, k, v, li, lf))
    b_cum, prev, final = mlstm_chunk_states(kc, vc, lic, lfc, state0)
    h = mlstm_chunk_outputs(qc, kc, vc, lic, b_cum, prev)
    return h.reshape(q.shape), final


def mlstm_final_state(k, v, li, lf, state0):
    return mlstm_chunk_states(to_chunks(k), to_chunks(v), to_chunks(li), to_chunks(lf), state0)[2]


def mlstm_merge(h_f, h_b, po, norm_g):
    b, _, l, _ = h_f.shape
    hs = h_f + h_b
    hs = hs * lax.rsqrt(jnp.mean(hs * hs, axis=-1, keepdims=True) + EPS)
    hs = hs.transpose(0, 2, 1, 3).reshape(b, l, GROUP_W) * norm_g.astype(jnp.float32)
    return (jax.nn.sigmoid(po.astype(jnp.float32)) * hs).astype(po.dtype)


def expert_choice_ffn(h, router_w, w1, w3, w2):
    b, n, _ = h.shape
    cap = CAPACITY_FACTOR * n // N_EXPERTS
    aff = jax.nn.softmax(jnp.einsum('bnd,de->bne', h, router_w).astype(jnp.float32), axis=-1)
    gate, idx = lax.top_k(jnp.swapaxes(aff, 1, 2), cap)
    bidx = jnp.arange(b)[:, None, None]
    xs = h[bidx, idx]
    hid = jax.nn.silu(jnp.einsum('becd,edf->becf', xs, w1)) * jnp.einsum('becd,edf->becf', xs, w3)
    ys = jnp.einsum('becf,efd->becd', hid, w2) * gate[..., None].astype(h.dtype)
    return jnp.zeros_like(h).at[bidx, idx].add(ys)


def local_mixers(p, conv_w, cmlp_norm_g, cmlp_ws, cmlp_bs):
    conv_out = short_conv_mixer(p['conv_x'], p['conv_b'], p['conv_c'], conv_w)
    cmlp_out = chunk_token_mlp(p['cmlp_u'], p['cmlp_v'], cmlp_norm_g, cmlp_ws, cmlp_bs)
    return conv_out, cmlp_out


def layer_forward(x, xc, c, c_ctx, cos, sin, ada_w, ada_b, norm1_g, w_in, conv_w, cmlp_norm_g, cmlp_ws,
                  cmlp_bs, q_norm_g, k_norm_g, ml_igate_b, ml_fgate_b, ml_norm_g, w_out, norm2_g, router_w,
                  exp_w1, exp_w3, exp_w2, update_ctx):
    bsz = x.shape[0]
    mod = (jax.nn.silu(c) @ ada_w + ada_b)[:, None, :]
    mod_c = (jax.nn.silu(c_ctx) @ ada_w + ada_b)[None, None, :]
    shift1, scale1, gate1, shift2, scale2, gate2 = jnp.split(mod, MOD_PARTS, axis=-1)
    cshift1, cscale1, cgate1, cshift2, cscale2, cgate2 = jnp.split(mod_c, MOD_PARTS, axis=-1)

    pc = split_in(modulate(rmsnorm(xc, norm1_g), cshift1, cscale1) @ w_in)
    qc, kc, vc = attn_qkv(pc['att_q'], pc['att_k'], pc['att_v'], q_norm_g, k_norm_g)
    mq_c, mk_c, mv_c, (li_cf, lf_cf), (li_cb, lf_cb) = mlstm_inputs(
        pc['ml_q'], pc['ml_k'], pc['ml_v'], pc['ml_i'], pc['ml_f'], ml_igate_b, ml_fgate_b)
    state0 = zero_state(bsz)
    if update_ctx:
        hcf, st_f = mlstm_scan(mq_c, mk_c, mv_c, li_cf, lf_cf, state0)
        hcb, st_b = mlstm_scan(flip_seq(mq_c), flip_seq(mk_c), flip_seq(mv_c), flip_seq(li_cb), flip_seq(lf_cb), state0)
        conv_c, cmlp_c = local_mixers(pc, conv_w, cmlp_norm_g, cmlp_ws, cmlp_bs)
        att_c = context_attention(qc, kc, vc)
        ml_c = mlstm_merge(hcf, flip_seq(hcb), pc['ml_o'], ml_norm_g)
        mix_c = jnp.concatenate([conv_c, cmlp_c, att_c.astype(xc.dtype), ml_c.astype(xc.dtype)], axis=-1) @ w_out
        xc_new = xc + cgate1 * mix_c
        hc2 = modulate(rmsnorm(xc_new, norm2_g), cshift2, cscale2)
        xc_new = xc_new + cgate2 * expert_choice_ffn(hc2, router_w, exp_w1, exp_w3, exp_w2)
    else:
        st_f = mlstm_final_state(mk_c, mv_c, li_cf, lf_cf, state0)
        st_b = mlstm_final_state(flip_seq(mk_c), flip_seq(mv_c), flip_seq(li_cb), flip_seq(lf_cb), state0)
        xc_new = xc

    p = split_in(modulate(rmsnorm(x, norm1_g), shift1, scale1) @ w_in)
    conv_x, cmlp_x = local_mixers(p, conv_w, cmlp_norm_g, cmlp_ws, cmlp_bs)
    q, k, v = attn_qkv(p['att_q'], p['att_k'], p['att_v'], q_norm_g, k_norm_g)
    q = apply_rope(q, cos, sin)
    k = apply_rope(k, cos, sin)
    att_x = latent_attention(q, k, v, kc, vc)
    mq, mk, mv, (li_f, lf_f), (li_b, lf_b) = mlstm_inputs(
        p['ml_q'], p['ml_k'], p['ml_v'], p['ml_i'], p['ml_f'], ml_igate_b, ml_fgate_b)
    hf, _ = mlstm_scan(mq, mk, mv, li_f, lf_f, st_f)
    hb, _ = mlstm_scan(flip_seq(mq), flip_seq(mk), flip_seq(mv), flip_seq(li_b), flip_seq(lf_b), st_b)
    ml_x = mlstm_merge(hf, flip_seq(hb), p['ml_o'], ml_norm_g)
    mix = jnp.concatenate([conv_x, cmlp_x, att_x.astype(x.dtype), ml_x.astype(x.dtype)], axis=-1) @ w_out
    x = x + gate1 * mix
    h2 = modulate(rmsnorm(x, norm2_g), shift2, scale2)
    x = x + gate2 * expert_choice_ffn(h2, router_w, exp_w1, exp_w3, exp_w2)
    return x, xc_new


def setup_inputs(seed: int = 0) -> dict:
    key = jax.random.key(seed)
    ks = jax.random.split(key, 24)
    f32 = jnp.float32
    L = DEPTH

    def nrm(k, shape, scale):
        return jax.random.normal(k, shape, f32) * scale

    def gain(k, shape):
        return 1.0 + 0.01 * jax.random.normal(k, shape, f32)

    return {
        'x': nrm(ks[0], (BATCH, SEQ, D_MODEL), 1.0),
        'c': nrm(ks[1], (BATCH, D_MODEL), 1.0),
        'ctx': nrm(ks[2], (BATCH, CTX_LEN, D_MODEL), 1.0),
        'c_ctx': nrm(ks[3], (D_MODEL,), 1.0),
        'ada_w': nrm(ks[4], (L, D_MODEL, MOD_PARTS * D_MODEL), 0.3 * D_MODEL ** -0.5),
        'ada_b': nrm(ks[5], (L, MOD_PARTS * D_MODEL), 0.01),
        'norm1_g': gain(ks[6], (L, D_MODEL)),
        'w_in': nrm(ks[7], (L, D_MODEL, D_IN), D_MODEL ** -0.5),
        'conv_w': nrm(ks[8], (L, CONV_W, GROUP_W), CONV_W ** -0.5),
        'cmlp_norm_g': gain(ks[9], (L, GROUP_W)),
        'cmlp_ws': nrm(ks[10], (L, CMLP_HEADS, CHUNK, CHUNK), CHUNK ** -0.5),
        'cmlp_bs': gain(ks[11], (L, CMLP_HEADS, CHUNK)),
        'q_norm_g': gain(ks[12], (L, ATT_HD)),
        'k_norm_g': gain(ks[13], (L, ATT_HD)),
        'ml_igate_b': nrm(ks[14], (L, 2 * ML_HEADS), 0.1),
        'ml_fgate_b': ML_FGATE_BIAS + nrm(ks[15], (L, 2 * ML_HEADS), 0.1),
        'ml_norm_g': gain(ks[16], (L, GROUP_W)),
        'w_out': nrm(ks[17], (L, D_MIX, D_MODEL), D_MIX ** -0.5),
        'norm2_g': gain(ks[18], (L, D_MODEL)),
        'router_w': nrm(ks[19], (L, D_MODEL, N_EXPERTS), D_MODEL ** -0.5),
        'exp_w1': nrm(ks[20], (L, N_EXPERTS, D_MODEL, EXPERT_FF), D_MODEL ** -0.5),
        'exp_w3': nrm(ks[21], (L, N_EXPERTS, D_MODEL, EXPERT_FF), D_MODEL ** -0.5),
        'exp_w2': nrm(ks[22], (L, N_EXPERTS, EXPERT_FF, D_MODEL), EXPERT_FF ** -0.5),
        'final_norm_g': gain(ks[23], (D_MODEL,)),
    }


def reference(x, c, ctx, c_ctx, ada_w, ada_b, norm1_g, w_in, conv_w, cmlp_norm_g, cmlp_ws, cmlp_bs,
              q_norm_g, k_norm_g, ml_igate_b, ml_fgate_b, ml_norm_g, w_out, norm2_g, router_w,
              exp_w1, exp_w3, exp_w2, final_norm_g):
    rows = x.shape[1] // GRID_W
    cos, sin = axial_rope_tables(rows)
    xc = ctx
    for l in range(DEPTH):
        x, xc = layer_forward(
            x, xc, c, c_ctx, cos, sin, ada_w[l], ada_b[l], norm1_g[l], w_in[l], conv_w[l], cmlp_norm_g[l],
            cmlp_ws[l], cmlp_bs[l], q_norm_g[l], k_norm_g[l], ml_igate_b[l], ml_fgate_b[l], ml_norm_g[l],
            w_out[l], norm2_g[l], router_w[l], exp_w1[l], exp_w3[l], exp_w2[l], update_ctx=(l < DEPTH - 1))
    return rmsnorm(x, final_norm_g)
```

```python
import numpy as np
import concourse.bass as bass
import concourse.mybir as mybir
from concourse.bass_utils import run_bass_kernel_spmd

F32 = mybir.dt.float32
BF16 = mybir.dt.bfloat16
I32 = mybir.dt.int32
AF = mybir.ActivationFunctionType
ALU = mybir.AluOpType
AX = mybir.AxisListType

NCORES = 8
D = 1024
SEQ = 16384
CTX = 256
TOKC = SEQ // NCORES
NTL = TOKC // 128
NT = NTL + 2
TOK = NT * 128
NE = 16
FF = 2048
CAP = 2 * SEQ // NE
CAPC = 2 * CTX // NE
EPS = 1e-6
NTM = 1552
NFM = 1536


class KB:
    NRING = 8

    def __init__(self):
        self.nc = nc = bass.Bass("TRN2", target_bir_lowering=False)
        self.E = {"pe": nc.tensor, "act": nc.scalar, "dve": nc.vector, "pool": nc.gpsimd, "sp": nc.sync}
        self.csem = {e: nc.alloc_semaphore("c_" + e) for e in ("pe", "act", "dve", "pool")}
        self.ccnt = {e: 0 for e in self.csem}
        self.dsem = {q: [nc.alloc_semaphore("d_%s%d" % (q, i)) for i in range(self.NRING)] for q in ("sp", "act", "pool")}
        self.dcnt = {q: [0] * self.NRING for q in self.dsem}
        self.dnext = {q: 0 for q in self.dsem}
        self.waited = {}
        self.lastw = {}
        self.readers = {}
        self.ccs = []
        self.n_inst = 0

    def sb(self, name, shape, dtype=F32):
        return self.nc.alloc_sbuf_tensor(name, list(shape), dtype)

    def ps(self, name, shape, dtype=F32):
        return self.nc.alloc_psum_tensor(name, list(shape), dtype)

    def dram(self, name, shape, dtype=F32, kind="Internal"):
        return self.nc.dram_tensor(name, list(shape), dtype, kind=kind)

    @staticmethod
    def key(x):
        if isinstance(x, str):
            return x
        if isinstance(x, tuple):
            return KB.key(x[0]) + ":" + str(x[1])
        return x.name

    def _wait(self, eng, tok):
        if tok is None:
            return
        sem, val = tok
        if val <= 0:
            return
        if eng == "pe" and sem.name.startswith("c_pe"):
            return
        k = (eng, sem.name)
        if self.waited.get(k, 0) >= val:
            return
        self.E[eng].wait_ge(sem, val)
        self.waited[k] = val
        self.n_inst += 1

    def _deps(self, eng, reads, writes):
        for r in reads:
            self._wait(eng, self.lastw.get(self.key(r)))
        for w in writes:
            k = self.key(w)
            self._wait(eng, self.lastw.get(k))
            for tok in self.readers.get(k, {}).values():
                self._wait(eng, tok)

    def _commit(self, tok, reads, writes):
        for r in reads:
            self.readers.setdefault(self.key(r), {})[tok[0].name] = tok
        for w in writes:
            k = self.key(w)
            self.lastw[k] = tok
            self.readers[k] = {}

    def op(self, eng, fn, reads=(), writes=(), inc=True):
        self._deps(eng, reads, writes)
        ins = fn(self.E[eng])
        self.n_inst += 1
        if inc:
            self.ccnt[eng] += 1
            ins.then_inc(self.csem[eng], 1)
            tok = (self.csem[eng], self.ccnt[eng])
            self._commit(tok, reads, writes)
            return tok
        return None

    def _dma_like(self, q, fn, reads, writes):
        slot = self.dnext[q]
        self.dnext[q] = (slot + 1) % self.NRING
        sem = self.dsem[q][slot]
        self._wait(q, (sem, self.dcnt[q][slot]))
        self._deps(q, reads, writes)
        ins = fn(self.E[q])
        self.dcnt[q][slot] += 16
        ins.then_inc(sem, 16)
        tok = (sem, self.dcnt[q][slot])
        self._commit(tok, reads, writes)
        self.n_inst += 1
        return tok

    def dma(self, q, out, in_, reads=(), writes=(), **kw):
        return self._dma_like(q, lambda e: e.dma_start(out=out, in_=in_, **kw), reads, writes)

    def gather(self, out, src, idx, bound, reads=(), writes=(), add=False):
        kw = {"compute_op": ALU.add} if add else {}
        return self._dma_like("pool", lambda e: e.indirect_dma_start(
            out=out, out_offset=None, in_=src, in_offset=bass.IndirectOffsetOnAxis(ap=idx, axis=0), **kw), reads, writes)

    def scatter(self, dst, src, idx, bound, reads=(), writes=(), add=False):
        kw = {"compute_op": ALU.add} if add else {}
        return self._dma_like("pool", lambda e: e.indirect_dma_start(
            out=dst, out_offset=bass.IndirectOffsetOnAxis(ap=idx, axis=0), in_=src, in_offset=None, **kw), reads, writes)

    def collective(self, kind, op, src, dst, reads, writes):
        sem = self.nc.alloc_semaphore("cc%d" % len(self.ccs))
        self.ccs.append(sem)
        self._deps("pool", reads, writes)
        ins = self.nc.gpsimd.collective_compute(kind, op, replica_groups=[list(range(NCORES))], ins=[src], outs=[dst])
        ins.then_inc(sem)
        tok = (sem, 1)
        self._commit(tok, reads, writes)
        self.n_inst += 1
        return tok

    def barrier(self):
        toks = []
        for q in self.dsem:
            for s in range(self.NRING):
                if self.dcnt[q][s]:
                    toks.append((self.dsem[q][s], self.dcnt[q][s]))
        for e in self.csem:
            if self.ccnt[e]:
                toks.append((self.csem[e], self.ccnt[e]))
        for sem in self.ccs:
            toks.append((sem, 1))
        for eng in ("pe", "act", "dve", "pool", "sp"):
            for t in toks:
                self._wait(eng, t)
        self.nbar = getattr(self, "nbar", 0) + 1
        for e in list(self.csem):
            if self.ccnt[e] > 20000:
                self.csem[e] = self.nc.alloc_semaphore("c_%s_%d" % (e, self.nbar))
                self.ccnt[e] = 0

    def finish(self):
        self.barrier()
        return self.nc


class Scope:
    def __init__(self, kb):
        from contextlib import ExitStack
        self.kb = kb
        self.st = ExitStack()

    _n = [0]

    def sb(self, name, shape, dtype=F32):
        Scope._n[0] += 1
        return self.st.enter_context(self.kb.nc.sbuf_tensor("%s_u%d" % (name, Scope._n[0]), list(shape), dtype))

    def close(self):
        self.kb.barrier()
        self.st.close()


NTOK = CTX + SEQ
NTILE = NTOK // 128
DIN = 2832
OFF = {}
_o = 0
for _n, _s in [('conv_x', 256), ('conv_b', 256), ('conv_c', 256), ('cmlp_u', 256), ('cmlp_v', 256), ('att_q', 256),
               ('att_k', 128), ('att_v', 128), ('ml_q', 256), ('ml_k', 256), ('ml_v', 256), ('ml_o', 256),
               ('ml_i', 8), ('ml_f', 8)]:
    OFF[_n] = _o
    _o += _s


import os
MLSTOP = int(os.environ.get('MLSTOP', '99'))


def build(nlayers=2, dbg=(), with_moe=True, phases="ABCDFGN"):
    kb = KB()
    nc = kb.nc
    I = lambda n, s, d=F32: kb.dram(n, s, d, "ExternalInput")
    xin = I("xin", [NTOK, D])
    cvec = I("cvec", [128, 8, 2])
    rope = I("rope", [NTOK, 128])
    consts = I("consts", [128, 10, 128])
    iota = I("iota", [128, 2048])
    W = []
    for l in range(2):
        w = {}
        for n, s in [('ada_w', [D, 6 * D]), ('ada_b', [1, 6 * D]), ('norm1_g', [D]), ('w_in', [D, DIN]),
                     ('conv_w', [3, 256]), ('cmlp_norm_g', [1, 256]), ('cmlp_ws', [4, 128, 128]), ('cmlp_bs', [4, 128]),
                     ('q_norm_g', [1, 64]), ('k_norm_g', [1, 64]), ('ml_gb', [1, 16]), ('ml_norm_g', [1, 256]),
                     ('w_out', [D, D]), ('norm2_g', [D]), ('router_w', [D, NE]), ('exp_w1', [NE, D, FF]),
                     ('exp_w3', [NE, D, FF]), ('exp_w2', [NE, FF, D])]:
            if n.startswith('exp_w') and not with_moe:
                continue
            w[n] = I("%s%d" % (n, l), s)
        W.append(w)
    fng = I("final_norm_g", [1, D])
    out = kb.dram("out", [SEQ, D], F32, "ExternalOutput")
    dbg_t = {}

    def dbgout(name, shape, dtype=F32):
        if name in dbg:
            dbg_t[name] = kb.dram("dbg_" + name, shape, dtype, "ExternalOutput")
            return dbg_t[name]
        return None

    z_d = kb.dram("z_d", [NTOK, DIN], F32)
    mix_d = kb.dram("mix_d", [NTOK, D], BF16)
    qT_d = kb.dram("qT_d", [128, 2, NTOK], BF16)
    kT_d = kb.dram("kT_d", [128, NTOK], BF16)
    v1_d = kb.dram("v1_d", [NTOK, 136], BF16)
    hf_d = kb.dram("hf_d", [NTOK, 256], F32)
    xn2_d = kb.dram("xn2_d", [NTOK, D], BF16)
    affT_d = kb.dram("affT_d", [NE, NTOK], F32)
    xs_d = [kb.dram("xs_d0", [NTOK, D], F32), kb.dram("xs_d1", [NTOK, D], F32)]
    modD = kb.dram("modD", [2, 6 * D], F32)
    pb = [kb.ps("pb%d" % i, [128, 512], F32) for i in range(8)]

    P = Scope(kb)
    cst = P.sb("cst", [128, 10, 128], F32)
    kb.dma("sp", cst[:], consts.ap(), writes=[cst])
    cstb = P.sb("cstb", [128, 10, 128], BF16)
    kb.op("dve", lambda e: e.tensor_copy(out=cstb[:], in_=cst[:]), reads=[cst], writes=[cstb])
    identf, triF, triB, onesf, triS = cst[:, 0, :], cst[:, 1, :], cst[:, 2, :], cst[:, 3, :], cst[:, 4, :]
    shP, shN = cstb[:, 5, :], cstb[:, 6, :]
    identb = cstb[:, 0, :]
    ePm, eNm = cstb[:, 7, :], cstb[:, 8, :]
    triFb, triBb = cstb[:, 1, :], cstb[:, 2, :]
    epst = P.sb("epst", [128, 1], F32)
    kb.op("pool", lambda e: e.memset(epst[:], EPS), writes=[epst])
    onet = P.sb("onet", [128, 1], F32)
    kb.op("pool", lambda e: e.memset(onet[:], 1.0), writes=[onet])
    modF = P.sb("modF", [128, 48, 2], F32)
    AB = P.sb("AB", [128, 2, 2, 8, 2], F32)
    gbc = P.sb("gbc", [128, 2, 2, D], F32)

    def rstd_of(ss, n_scale_done=True):
        kb.op("act", lambda e: e.activation(out=ss, in_=ss, func=AF.Sqrt, bias=epst[0:ss.shape[0], :], scale=1.0),
              reads=[ss.tensor if hasattr(ss, 'tensor') else ss, epst], writes=[ss.tensor if hasattr(ss, 'tensor') else ss])

    cur = xin
    for l in range(nlayers):
        w = W[l]
        xnext = xs_d[l]
        S = Scope(kb)
        sc = S.sb("sc", [128, 8, 2], F32)
        kb.dma("sp", sc[:], cvec.ap(), writes=[sc])
        kb.op("act", lambda e: e.activation(out=sc[:], in_=sc[:], func=AF.Silu), reads=[sc], writes=[sc])
        modrow = S.sb("modrow", [2, 6 * D], F32)
        adab = S.sb("adab", [2, 6 * D], F32)
        kb.dma("sp", adab[:], w['ada_b'].ap().partition_broadcast(2), writes=[adab])
        wbufs = [S.sb("adaw%d" % i, [128, 8, 512], F32) for i in range(2)]
        for n in range(12):
            wb = wbufs[n % 2]
            kb.dma("sp", wb[:], w['ada_w'].ap()[:, n * 512:(n + 1) * 512].rearrange("(kc p) n -> p kc n", p=128), writes=[wb])
            bank = pb[n % 2]
            for kc in range(8):
                kb.op("pe", lambda e, kc=kc: e.matmul(bank[0:2, :], lhsT=sc[:, kc, :], rhs=wb[:, kc, :], start=(kc == 0), stop=(kc == 7)),
                      reads=[sc, wb], writes=[bank], inc=(kc == 7))
            kb.op("dve", lambda e: e.tensor_tensor(out=modrow[:, n * 512:(n + 1) * 512], in0=bank[0:2, :], in1=adab[:, n * 512:(n + 1) * 512], op=ALU.add),
                  reads=[bank, adab], writes=[modrow])
        kb.dma("pool", modD.ap(), modrow[:], reads=[modrow], writes=[modD])
        bank = pb[2]
        for m in range(48):
            kb.op("pe", lambda e, m=m: e.transpose(out=bank[:, 2 * m:2 * m + 2], in_=modrow[0:2, m * 128:(m + 1) * 128], identity=cst[0:2, 0, 0:2]),
                  reads=[modrow, cst], writes=[bank], inc=(m == 47))
        kb.op("dve", lambda e: e.tensor_copy(out=modF[:].rearrange("p m j -> p (m j)"), in_=bank[:, 0:96]), reads=[bank], writes=[modF])
        g12 = S.sb("g12", [128, 2, 8], F32)
        kb.dma("sp", g12[:, 0, :], w['norm1_g'].ap().rearrange("(kc p) -> p kc", p=128), writes=[g12], allow_slow_non_contiguous=True)
        kb.dma("sp", g12[:, 1, :], w['norm2_g'].ap().rearrange("(kc p) -> p kc", p=128), writes=[g12], allow_slow_non_contiguous=True)
        for s in range(2):
            sh, scl, gt = (0, 8, 16) if s == 0 else (24, 32, 40)
            for j in range(2):
                kb.op("dve", lambda e, s=s, j=j, scl=scl: e.scalar_tensor_tensor(out=AB[:, s, 0, :, j], in0=modF[:, scl:scl + 8, j], scalar=1.0, in1=g12[:, s, :], op0=ALU.add, op1=ALU.mult),
                      reads=[modF, g12], writes=[AB])
                kb.op("dve", lambda e, s=s, j=j, sh=sh: e.tensor_copy(out=AB[:, s, 1, :, j], in_=modF[:, sh:sh + 8, j]), reads=[modF], writes=[AB])
                kb.dma("sp", gbc[:, s, j, :], modD.ap()[j:j + 1, gt * 128:(gt + 8) * 128].partition_broadcast(128), reads=[modD], writes=[gbc])
        S.close()

        def norm_T(S_, xt, sub, j, hT, tag):
            ss = S_["ss"]; junk = S_["junk"]; xn = S_["xn"]
            kb.op("act", lambda e: e.activation(out=junk[:], in_=xt[:], func=AF.Square, scale=1.0 / 32.0, accum_out=ss[:]), reads=[xt], writes=[junk, ss])
            kb.op("act", lambda e: e.activation(out=ss[:], in_=ss[:], func=AF.Sqrt, bias=epst[:], scale=1.0), reads=[ss, epst], writes=[ss])
            kb.op("dve", lambda e: e.reciprocal(out=ss[:], in_=ss[:]), reads=[ss], writes=[ss])
            kb.op("pool", lambda e: e.tensor_scalar(out=xn[:], in0=xt[:], scalar1=ss[:], scalar2=None, op0=ALU.mult), reads=[xt, ss], writes=[xn])
            tp = pb[0]
            tpb = tp[:].bitcast(BF16)
            for kc in range(8):
                kb.op("pe", lambda e, kc=kc: e.transpose(out=tpb[:, kc * 128:(kc + 1) * 128], in_=xn[:, kc * 128:(kc + 1) * 128], identity=identb),
                      reads=[xn, cstb], writes=[tp], inc=(kc == 7))
            for kc in range(8):
                if kc % 2 == 0:
                    kb.op("dve", lambda e, kc=kc: e.tensor_scalar(out=hT[:, kc, :], in0=tpb[:, kc * 128:(kc + 1) * 128], scalar1=AB[:, sub, 0, kc, j:j + 1], scalar2=AB[:, sub, 1, kc, j:j + 1], op0=ALU.mult, op1=ALU.add),
                          reads=[tp, AB], writes=[hT])
                else:
                    kb.op("act", lambda e, kc=kc: e.activation(out=hT[:, kc, :], in_=tpb[:, kc * 128:(kc + 1) * 128], func=AF.Identity, scale=AB[:, sub, 0, kc, j:j + 1], bias=AB[:, sub, 1, kc, j:j + 1]),
                          reads=[tp, AB], writes=[hT])

        S = Scope(kb)
        w_sb = S.sb("w_sb", [128, 8, DIN], BF16)
        for kc in range(8):
            kb.dma("pool", w_sb[:, kc, :], w['w_in'].ap()[kc * 128:(kc + 1) * 128, :], writes=[w_sb])
        wsT = S.sb("wsT", [128, 4, 128], BF16)
        wsf = S.sb("wsf", [128, 4, 128], F32)
        kb.dma("sp", wsf[:], w['cmlp_ws'].ap().rearrange("h t s -> t h s"), writes=[wsf])
        for h in range(4):
            kb.op("pe", lambda e, h=h: e.transpose(out=pb[1][:, h * 128:(h + 1) * 128], in_=wsf[:, h, :], identity=identf), reads=[wsf, cst], writes=[pb[1]], inc=(h == 3))
        kb.op("dve", lambda e: e.tensor_copy(out=wsT[:].rearrange("p h t -> p (h t)"), in_=pb[1][:, :]), reads=[pb[1]], writes=[wsT])
        bsT = S.sb("bsT", [128, 4], F32)
        kb.dma("sp", bsT[:], w['cmlp_bs'].ap().rearrange("h t -> t h"), writes=[bsT], allow_slow_non_contiguous=True)
        gv = S.sb("gv", [128, 256], F32)
        kb.dma("sp", gv[:], w['cmlp_norm_g'].ap().partition_broadcast(128), writes=[gv])
        gqk = S.sb("gqk", [128, 2, 64], F32)
        kb.dma("sp", gqk[:, 0, :], w['q_norm_g'].ap().partition_broadcast(128), writes=[gqk])
        kb.dma("sp", gqk[:, 1, :], w['k_norm_g'].ap().partition_broadcast(128), writes=[gqk])
        bufs = []
        for i in range(2):
            bufs.append({n: S.sb("%s_%d" % (n, i), shp, dt) for n, shp, dt in [
                ("xt", [128, D], F32), ("junk", [128, D], F32), ("xn", [128, D], BF16), ("ss", [128, 1], F32),
                ("hT", [128, 8, 128], BF16), ("zt", [128, DIN], F32), ("rp", [128, 128], F32), ("qk", [128, 6, 64], F32),
                ("sq", [128, 6, 64], F32), ("s6", [128, 6], F32), ("r1", [128, 6, 64], F32), ("r2", [128, 6, 64], F32),
                ("qkb", [128, 384], BF16), ("qkT", [128, 384], BF16), ("v1", [128, 136], BF16), ("vn", [128, 256], BF16),
                ("sv", [128, 1], F32), ("cm", [128, 256], BF16)]})
        for t in range(NTILE if 'A' in phases else 0):
            B = bufs[t % 2]
            j = 1 if t < 2 else 0
            xt, hT, zt = B["xt"], B["hT"], B["zt"]
            kb.dma("sp", xt[:], cur.ap()[t * 128:(t + 1) * 128, :], reads=[cur], writes=[xt])
            kb.dma("sp", B["rp"][:], rope.ap()[t * 128:(t + 1) * 128, :], writes=[B["rp"]])
            norm_T(B, xt, 0, j, hT, "a")
            for ci in range(6):
                c0 = ci * 512
                c1 = min(DIN, c0 + 512)
                bank = pb[1 + ci % 2]
                for kc in range(8):
                    kb.op("pe", lambda e, kc=kc: e.matmul(bank[:, 0:c1 - c0], lhsT=hT[:, kc, :], rhs=w_sb[:, kc, c0:c1], start=(kc == 0), stop=(kc == 7)),
                          reads=[hT, w_sb], writes=[bank], inc=(kc == 7))
                if ci % 2 == 0:
                    kb.op("act", lambda e: e.copy(out=zt[:, c0:c1], in_=bank[:, 0:c1 - c0]), reads=[bank], writes=[zt])
                else:
                    kb.op("dve", lambda e: e.tensor_copy(out=zt[:, c0:c1], in_=bank[:, 0:c1 - c0]), reads=[bank], writes=[zt])
            kb.dma("pool", z_d.ap()[t * 128:(t + 1) * 128, :], zt[:], reads=[zt], writes=[z_d])
            qk, sq, s6, r1, r2, rp = B["qk"], B["sq"], B["s6"], B["r1"], B["r2"], B["rp"]
            zqk = zt[:, OFF['att_q']:OFF['att_q'] + 384].rearrange("p (h d) -> p h d", d=64)
            kb.op("pool", lambda e: e.tensor_tensor(out=sq[:], in0=zqk, in1=zqk, op=ALU.mult), reads=[zt], writes=[sq])
            kb.op("dve", lambda e: e.tensor_reduce(out=s6[:], in_=sq[:], axis=AX.X, op=ALU.add), reads=[sq], writes=[s6])
            kb.op("act", lambda e: e.activation(out=s6[:], in_=s6[:], func=AF.Sqrt, bias=epst[:], scale=1.0 / 64.0), reads=[s6, epst], writes=[s6])
            kb.op("dve", lambda e: e.reciprocal(out=s6[:], in_=s6[:]), reads=[s6], writes=[s6])
            kb.op("dve", lambda e: e.tensor_tensor(out=qk[:], in0=zqk, in1=s6[:, :, None].to_broadcast([128, 6, 64]), op=ALU.mult), reads=[zt, s6], writes=[qk])
            kb.op("pool", lambda e: e.tensor_tensor(out=qk[:, 0:4, :], in0=qk[:, 0:4, :], in1=gqk[:, 0:1, :].to_broadcast([128, 4, 64]), op=ALU.mult), reads=[qk, gqk], writes=[qk])
            kb.op("pool", lambda e: e.tensor_tensor(out=qk[:, 4:6, :], in0=qk[:, 4:6, :], in1=gqk[:, 1:2, :].to_broadcast([128, 2, 64]), op=ALU.mult), reads=[qk, gqk], writes=[qk])
            cosb = rp[:, 0:64][:, None, :].to_broadcast([128, 6, 64])
            kb.op("dve", lambda e: e.tensor_tensor(out=r1[:], in0=qk[:], in1=cosb, op=ALU.mult), reads=[qk, rp], writes=[r1])
            qv = qk[:].rearrange("p h (a q i) -> p (h a) q i", a=2, q=2)
            r2v = r2[:].rearrange("p h (a q i) -> p (h a) q i", a=2, q=2)
            sv_ = rp[:, 64:128].rearrange("p (a q i) -> p a q i", a=2, q=2)
            for a in range(2):
                for qq in range(2):
                    kb.op("pool", lambda e, a=a, qq=qq: e.tensor_tensor(
                        out=r2[:].rearrange("p h (a q i) -> p h a q i", a=2, q=2)[:, :, a, qq, :],
                        in0=qk[:].rearrange("p h (a q i) -> p h a q i", a=2, q=2)[:, :, a, 1 - qq, :],
                        in1=sv_[:, a, qq, :][:, None, :].to_broadcast([128, 6, 16]), op=ALU.mult), reads=[qk, rp], writes=[r2])
            qkb = B["qkb"]
            kb.op("dve", lambda e: e.tensor_tensor(out=qkb[:].rearrange("p (h d) -> p h d", d=64), in0=r1[:], in1=r2[:], op=ALU.add), reads=[r1, r2], writes=[qkb])
            tq = pb[3]
            tqb = tq[:].bitcast(BF16)
            qkbv = qkb[:, 0:256].rearrange("p (g j d) -> p j g d", g=2, j=2)
            qkT = B["qkT"]
            for jj in range(2):
                kb.op("pool", lambda e, jj=jj: e.tensor_copy(out=qkT[:, jj * 128:(jj + 1) * 128].rearrange("p (g d) -> p g d", g=2), in_=qkbv[:, jj, :, :]), reads=[qkb], writes=[qkT])
            kb.op("pool", lambda e: e.tensor_copy(out=qkT[:, 256:384], in_=qkb[:, 256:384]), reads=[qkb], writes=[qkT])
            for i3 in range(3):
                kb.op("pe", lambda e, i3=i3: e.transpose(out=tqb[:, i3 * 128:(i3 + 1) * 128], in_=qkT[:, i3 * 128:(i3 + 1) * 128], identity=identb), reads=[qkT, cstb], writes=[tq], inc=(i3 == 2))
            kb.op("act", lambda e: e.copy(out=qkb[:], in_=tqb[:, 0:384]), reads=[tq], writes=[qkb])
            kb.dma("pool", qT_d.ap()[:, :, t * 128:(t + 1) * 128], qkb[:, 0:256].rearrange("p (j t) -> p j t", j=2), reads=[qkb], writes=[qT_d])
            kb.dma("pool", kT_d.ap()[:, t * 128:(t + 1) * 128], qkb[:, 256:384], reads=[qkb], writes=[kT_d])
            v1 = B["v1"]
            kb.op("pool", lambda e: e.memset(v1[:], 1.0), writes=[v1])
            kb.op("act", lambda e: e.copy(out=v1[:].rearrange("p (g c) -> p g c", g=2)[:, :, 0:64], in_=zt[:, OFF['att_v']:OFF['att_v'] + 128].rearrange("p (g c) -> p g c", g=2)), reads=[zt], writes=[v1])
            kb.dma("pool", v1_d.ap()[t * 128:(t + 1) * 128, :], v1[:], reads=[v1], writes=[v1_d])
            vn, sv1, cm = B["vn"], B["sv"], B["cm"]
            kb.op("act", lambda e: e.activation(out=B["junk"][:, 0:256], in_=zt[:, OFF['cmlp_v']:OFF['cmlp_v'] + 256], func=AF.Square, scale=1.0 / 16.0, accum_out=sv1[:]), reads=[zt], writes=[B["junk"], sv1])
            kb.op("act", lambda e: e.activation(out=sv1[:], in_=sv1[:], func=AF.Sqrt, bias=epst[:], scale=1.0), reads=[sv1, epst], writes=[sv1])
            kb.op("dve", lambda e: e.reciprocal(out=sv1[:], in_=sv1[:]), reads=[sv1], writes=[sv1])
            kb.op("dve", lambda e: e.scalar_tensor_tensor(out=vn[:], in0=zt[:, OFF['cmlp_v']:OFF['cmlp_v'] + 256], scalar=sv1[:], in1=gv[:], op0=ALU.mult, op1=ALU.mult), reads=[zt, sv1, gv], writes=[vn])
            bank = pb[4 + t % 2]
            for h in range(4):
                kb.op("pe", lambda e, h=h: e.matmul(bank[:, h * 64:(h + 1) * 64], lhsT=wsT[:, h, :], rhs=vn[:, h * 64:(h + 1) * 64], start=True, stop=True), reads=[wsT, vn], writes=[bank], inc=(h == 3))
            for h in range(4):
                kb.op("dve", lambda e, h=h: e.scalar_tensor_tensor(out=cm[:, h * 64:(h + 1) * 64], in0=bank[:, h * 64:(h + 1) * 64], scalar=bsT[:, h:h + 1], in1=zt[:, OFF['cmlp_u'] + h * 64:OFF['cmlp_u'] + (h + 1) * 64], op0=ALU.add, op1=ALU.mult), reads=[bank, bsT, zt], writes=[cm])
            kb.dma("pool", mix_d.ap()[t * 128:(t + 1) * 128, 256:512], cm[:], reads=[cm], writes=[mix_d])
        S.close()
        if l == 0:
            d_ = dbgout("z", [NTOK, DIN])
            if d_ is not None:
                kb.dma("sp", d_.ap(), z_d.ap(), reads=[z_d], writes=[d_])
            d_ = dbgout("kT", [128, NTOK], BF16)
            if d_ is not None:
                kb.dma("sp", d_.ap(), kT_d.ap(), reads=[kT_d], writes=[d_])
            d_ = dbgout("qT", [128, 2, NTOK], BF16)
            if d_ is not None:
                kb.dma("sp", d_.ap(), qT_d.ap(), reads=[qT_d], writes=[d_])
            d_ = dbgout("mix", [NTOK, D], BF16)
            if d_ is not None:
                kb.dma("sp", d_.ap(), mix_d.ap(), reads=[mix_d], writes=[d_])

        S = Scope(kb)
        NT_B = NTILE if 'B' in phases else -1
        cwr = S.sb("cwr", [128, 3, 256], F32)
        for i3 in range(3):
            kb.dma("sp", cwr[:, i3, :], w['conv_w'].ap()[i3:i3 + 1, :].partition_broadcast(128), writes=[cwr])
        zc = [S.sb("zc%d" % i, [128, 768], F32) for i in range(3)]
        yb = [S.sb("yb%d" % i, [128, 256], BF16) for i in range(3)]
        cacc = [S.sb("cacc%d" % i, [128, 256], F32) for i in range(2)]
        ctmp = [S.sb("ctmp%d" % i, [128, 2, 256], F32) for i in range(2)]
        cob = [S.sb("cob%d" % i, [128, 256], BF16) for i in range(2)]
        for i in range(NT_B + 1):
            if i < NTILE:
                zt_, y_ = zc[i % 3], yb[i % 3]
                kb.dma("sp", zt_[:], z_d.ap()[i * 128:(i + 1) * 128, 0:768], reads=[z_d], writes=[zt_])
                kb.op("pool", lambda e, zt_=zt_, y_=y_: e.tensor_tensor(out=y_[:], in0=zt_[:, 0:256], in1=zt_[:, 512:768], op=ALU.mult), reads=[zt_], writes=[y_])
            t = i - 1
            if t < 0:
                continue
            has_prev = t not in (0, 2)
            has_next = t not in (1, NTILE - 1)
            y_, zt_ = yb[t % 3], zc[t % 3]
            bp, bn = pb[1], pb[2]
            kb.op("pe", lambda e: e.matmul(bp[:, 0:256], lhsT=shP, rhs=y_[:], start=True, stop=not has_prev), reads=[cstb, y_], writes=[bp], inc=not has_prev)
            if has_prev:
                yp = yb[(t - 1) % 3]
                kb.op("pe", lambda e: e.matmul(bp[:, 0:256], lhsT=ePm, rhs=yp[:], start=False, stop=True), reads=[cstb, yp, y_], writes=[bp])
            kb.op("pe", lambda e: e.matmul(bn[:, 0:256], lhsT=shN, rhs=y_[:], start=True, stop=not has_next), reads=[cstb, y_], writes=[bn], inc=not has_next)
            if has_next:
                yn = yb[(t + 1) % 3]
                kb.op("pe", lambda e: e.matmul(bn[:, 0:256], lhsT=eNm, rhs=yn[:], start=False, stop=True), reads=[cstb, yn, y_], writes=[bn])
            ca, ct, co = cacc[t % 2], ctmp[t % 2], cob[t % 2]
            kb.op("dve", lambda e: e.tensor_tensor(out=ct[:, 0, :], in0=bp[:, 0:256], in1=cwr[:, 0, :], op=ALU.mult), reads=[bp, cwr], writes=[ct])
            kb.op("dve", lambda e: e.tensor_tensor(out=ct[:, 1, :], in0=bn[:, 0:256], in1=cwr[:, 2, :], op=ALU.mult), reads=[bn, cwr], writes=[ct])
            kb.op("pool", lambda e: e.tensor_tensor(out=ca[:], in0=y_[:], in1=cwr[:, 1, :], op=ALU.mult), reads=[y_, cwr], writes=[ca])
            kb.op("pool", lambda e: e.tensor_tensor(out=ca[:], in0=ca[:], in1=ct[:, 0, :], op=ALU.add), reads=[ca, ct], writes=[ca])
            kb.op("pool", lambda e: e.tensor_tensor(out=ca[:], in0=ca[:], in1=ct[:, 1, :], op=ALU.add), reads=[ca, ct], writes=[ca])
            kb.op("pool", lambda e: e.tensor_tensor(out=co[:], in0=ca[:], in1=zt_[:, 256:512], op=ALU.mult), reads=[ca, zt_], writes=[co])
            kb.dma("pool", mix_d.ap()[t * 128:(t + 1) * 128, 0:256], co[:], reads=[co], writes=[mix_d])
        S.close()

        S = Scope(kb)
        KT = S.sb("KT", [128, NTOK], BF16)
        kb.dma("sp", KT[:], kT_d.ap(), reads=[kT_d], writes=[KT])
        V1 = S.sb("V1", [128, NTILE, 136], BF16)
        for i10 in range(10):
            kb.dma("sp", V1[:, i10 * 13:(i10 + 1) * 13, :], v1_d.ap()[i10 * 13 * 128:(i10 + 1) * 13 * 128, :].rearrange("(n p) c -> p n c", p=128), reads=[v1_d], writes=[V1])
        qcb = [S.sb("qc%d" % i, [128, 512], BF16) for i in range(2)]
        Pb = [S.sb("Pb%d" % i, [128, 512], BF16) for i in range(3)]
        rec = [S.sb("rec%d" % i, [128, 1], F32) for i in range(4)]
        ot = [S.sb("ot%d" % i, [128, 64], BF16) for i in range(4)]
        chunks = [(0, 256, 2)] + [(CTX + i * 512, 512, NTILE) for i in range(SEQ // 512)]
        ci_ = 0
        for h in range(4 if 'C' in phases else 0):
            g, jj = h // 2, h % 2
            gs = slice(g * 64, (g + 1) * 64)
            for (q0, nq, nk) in chunks:
                qc = qcb[ci_ % 2]
                ci_ += 1
                kb.dma("sp", qc[gs, 0:nq], qT_d.ap()[gs, jj, q0:q0 + nq], reads=[qT_d], writes=[qc])
                nqt = nq // 128

                def qk_mm(kt):
                    sb_ = pb[kt % 3]
                    kb.op("pe", lambda e: e.matmul(sb_[:, 0:nq], lhsT=KT[gs, kt * 128:(kt + 1) * 128], rhs=qc[gs, 0:nq], start=True, stop=True), reads=[KT, qc], writes=[sb_])
                qk_mm(0)
                if nk > 1:
                    qk_mm(1)
                for kt in range(nk):
                    sb_, p_ = pb[kt % 3], Pb[kt % 3]
                    kb.op("act", lambda e: e.activation(out=p_[:, 0:nq], in_=sb_[:, 0:nq], func=AF.Exp, scale=0.125), reads=[sb_], writes=[p_])
                    if kt + 2 < nk:
                        qk_mm(kt + 2)
                    for qi in range(nqt):
                        acc = pb[4 + qi]
                        kb.op("pe", lambda e, qi=qi, acc=acc: e.matmul(acc[:, 0:65], lhsT=p_[:, qi * 128:(qi + 1) * 128], rhs=V1[:, kt, g * 68:g * 68 + 65], start=(kt == 0), stop=(kt == nk - 1)),
                              reads=[p_, V1], writes=[acc], inc=(qi == nqt - 1))
                        if qi == nqt - 1:
                            kb._commit((kb.csem["pe"], kb.ccnt["pe"]), [p_, V1], [pb[4 + x] for x in range(nqt)])
                for qi in range(nqt):
                    acc = pb[4 + qi]
                    kb.op("dve", lambda e, qi=qi, acc=acc: e.reciprocal(out=rec[qi][:], in_=acc[:, 64:65]), reads=[acc], writes=[rec[qi]])
                    kb.op("act", lambda e, qi=qi, acc=acc: e.activation(out=ot[qi][:], in_=acc[:, 0:64], func=AF.Copy, scale=rec[qi][:]), reads=[acc, rec[qi]], writes=[ot[qi]])
                    kb.dma("pool", mix_d.ap()[q0 + qi * 128:q0 + (qi + 1) * 128, 512 + h * 64:512 + (h + 1) * 64], ot[qi][:], reads=[ot[qi]], writes=[mix_d])
        S.close()

        S = Scope(kb)
        gb = S.sb("gb", [128, 16], F32)
        kb.dma("sp", gb[:], w['ml_gb'].ap().partition_broadcast(128), writes=[gb])
        gml = S.sb("gml", [128, 256], F32)
        kb.dma("sp", gml[:], w['ml_norm_g'].ap().partition_broadcast(128), writes=[gml])
        MB = []
        for i in range(2):
            MB.append({n: S.sb("%s_%d" % (n, i), shp, dt) for n, shp, dt in [
                ("zm", [128, 1040], F32), ("xg", [128, 4], F32), ("lg", [128, 4], F32), ("cc", [128, 4], F32), ("aa", [128, 4], F32),
                ("ebl", [128, 4], F32), ("qb", [128, 256], BF16), ("kbm", [128, 256], BF16), ("qT", [128, 2, 128], BF16),
                ("kT0", [128, 2, 128], BF16), ("kT1", [128, 2, 128], BF16), ("av", [128, 4, 68], BF16), ("Pm", [128, 4, 128], BF16), ("cd", [128, 4], F32),
                ("fac", [128, 4], F32), ("hd", [128, 4, 64], F32), ("hf", [128, 4, 64], F32), ("sq", [128, 4, 64], F32),
                ("s4", [128, 4], F32), ("sg", [128, 256], F32), ("mo", [128, 256], BF16), ("tmpS", [128, 2, 65], F32)]})
        St = S.sb("St", [128, 2, 68], F32)
        Sb = [S.sb("Sb0", [128, 2, 68], BF16), S.sb("Sb1", [128, 2, 68], BF16)]
        for i in range(2):
            for nm in ("kT0", "kT1"):
                kb.op("pool", lambda e, i=i, nm=nm: e.memset(MB[i][nm][:], 0.0), writes=[MB[i][nm]])
        for d in range(2 if 'D' in phases else 0):
            order = list(range(NTILE)) if d == 0 else [1, 0] + list(range(NTILE - 1, 1, -1))
            tri = triF if d == 0 else triB
            kb.op("pool", lambda e: e.memset(St[:], 0.0), writes=[St])
            kb.op("pool", lambda e: e.memset(Sb[0][:], 0.0), writes=[Sb[0]])
            kb.op("pool", lambda e: e.memset(Sb[1][:], 0.0), writes=[Sb[1]])
            for it, t in enumerate(order):
                B = MB[it % 2]
                zm = B["zm"]
                kb.dma("sp", zm[:], z_d.ap()[t * 128:(t + 1) * 128, OFF['ml_q']:OFF['ml_q'] + 1040], reads=[z_d], writes=[zm])
                if d == 1:
                    kb.dma("sp", B["hf"][:].rearrange("p h d -> p (h d)"), hf_d.ap()[t * 128:(t + 1) * 128, :], reads=[hf_d], writes=[B["hf"]])
                xg, lg, cc, aa, ebl = B["xg"], B["lg"], B["cc"], B["aa"], B["ebl"]
                kb.op("dve", lambda e: e.tensor_tensor(out=xg[:], in0=zm[:, 1032 + 4 * d:1036 + 4 * d], in1=gb[:, 8 + 4 * d:12 + 4 * d], op=ALU.add), reads=[zm, gb], writes=[xg])
                kb.op("act", lambda e: e.activation(out=lg[:], in_=xg[:], func=AF.Exp, scale=-1.0), reads=[xg], writes=[lg])
                kb.op("act", lambda e: e.activation(out=lg[:], in_=lg[:], func=AF.Ln, bias=onet[:], scale=1.0), reads=[lg, onet], writes=[lg])
                bk = pb[0]
                kb.op("pe", lambda e: e.matmul(bk[:, 0:4], lhsT=tri, rhs=lg[:], start=True, stop=True), reads=[cst, lg], writes=[bk], inc=False)
                kb.op("pe", lambda e: e.matmul(bk[:, 4:8], lhsT=onesf, rhs=lg[:], start=True, stop=True), reads=[cst, lg], writes=[bk])
                kb.op("act", lambda e: e.activation(out=cc[:], in_=bk[:, 0:4], func=AF.Exp, scale=-1.0), reads=[bk], writes=[cc])
                kb.op("act", lambda e: e.activation(out=ebl[:], in_=bk[:, 4:8], func=AF.Exp, scale=-1.0), reads=[bk], writes=[ebl])
                kb.op("dve", lambda e: e.tensor_tensor(out=aa[:], in0=zm[:, 1024 + 4 * d:1028 + 4 * d], in1=gb[:, 4 * d:4 * d + 4], op=ALU.add), reads=[zm, gb], writes=[aa])
                kb.op("dve", lambda e: e.tensor_tensor(out=aa[:], in0=aa[:], in1=bk[:, 0:4], op=ALU.add), reads=[aa, bk], writes=[aa])
                kb.op("act", lambda e: e.activation(out=aa[:], in_=aa[:], func=AF.Exp), reads=[aa], writes=[aa])
                if MLSTOP == 1:
                    continue
                qb, kbm, qT, kTz, av, Pm = B["qb"], B["kbm"], B["qT"], [B["kT0"], B["kT1"]], B["av"], B["Pm"]
                kb.op("pool", lambda e: e.tensor_copy(out=qb[:], in_=zm[:, 0:256]), reads=[zm], writes=[qb])
                kb.op("pool", lambda e: e.tensor_scalar(out=kbm[:], in0=zm[:, 256:512], scalar1=0.125, scalar2=None, op0=ALU.mult), reads=[zm], writes=[kbm])
                tq = pb[1]
                tqb = tq[:].bitcast(BF16)
                for c in range(2):
                    kb.op("pe", lambda e, c=c: e.transpose(out=tqb[:, c * 128:(c + 1) * 128], in_=qb[:, c * 128:(c + 1) * 128], identity=identb), reads=[qb, cstb], writes=[tq], inc=False)
                    kb.op("pe", lambda e, c=c: e.transpose(out=tqb[:, 256 + c * 128:256 + (c + 1) * 128], in_=kbm[:, c * 128:(c + 1) * 128], identity=identb), reads=[kbm, cstb], writes=[tq], inc=(c == 1))
                kb.commit = None
                kb._commit((kb.csem["pe"], kb.ccnt["pe"]), [qb, kbm], [tq])
                kb.op("act", lambda e: e.copy(out=qT[:].rearrange("p c t -> p (c t)"), in_=tqb[:, 0:256]), reads=[tq], writes=[qT])
                kb.op("act", lambda e: e.copy(out=kTz[0][0:64, :, :].rearrange("p c t -> p (c t)"), in_=tqb[0:64, 256:512]), reads=[tq], writes=[kTz[0]])
                kb.op("act", lambda e: e.copy(out=kTz[1][64:128, :, :].rearrange("p c t -> p (c t)"), in_=tqb[64:128, 256:512]), reads=[tq], writes=[kTz[1]])
                kb.op("dve", lambda e: e.tensor_tensor(out=av[:, :, 0:64], in0=zm[:, 512:768].rearrange("p (h d) -> p h d", d=64), in1=aa[:, :, None].to_broadcast([128, 4, 64]), op=ALU.mult), reads=[zm, aa], writes=[av])
                kb.op("dve", lambda e: e.tensor_copy(out=av[:, :, 64], in_=aa[:]), reads=[aa], writes=[av])
                if MLSTOP == 2:
                    continue
                kq = pb[2]
                for h in range(4):
                    c, par = h // 2, h % 2
                    ps_ = slice(par * 64, (par + 1) * 64)
                    kb.op("pe", lambda e, h=h, c=c, ps_=ps_: e.matmul(kq[:, h * 128:(h + 1) * 128], lhsT=kTz[h % 2][:, c, :], rhs=qT[:, c, :], start=True, stop=True), reads=[kTz[0], kTz[1], qT], writes=[kq], inc=(h == 3))
                kb._commit((kb.csem["pe"], kb.ccnt["pe"]), [kTz[0], kTz[1], qT], [kq])
                if MLSTOP == 25:
                    continue
                kb.op("dve", lambda e: e.tensor_tensor(out=Pm[:], in0=kq[:, :].rearrange("p (h t) -> p h t", h=4), in1=tri[:, None, :].to_broadcast([128, 4, 128]), op=ALU.mult), reads=[kq, cst], writes=[Pm])
                if MLSTOP == 3:
                    continue
                ob = pb[3]
                for h in range(4):
                    c, par = h // 2, h % 2
                    ps_ = slice(par * 64, (par + 1) * 64)
                    kb.op("pe", lambda e, h=h: e.matmul(ob[:, h * 68:h * 68 + 65], lhsT=Pm[:, h, :], rhs=av[:, h, 0:65], start=True, stop=False), reads=[Pm, av], writes=[ob], inc=False)
                    kb.op("pe", lambda e, h=h, c=c, ps_=ps_: e.matmul(ob[:, h * 68:h * 68 + 65], lhsT=qT[:, c, :], rhs=Sb[h % 2][:, c, 0:65], start=False, stop=True), reads=[qT, Sb[0], Sb[1], Pm, av], writes=[ob], inc=(h == 3))
                kb._commit((kb.csem["pe"], kb.ccnt["pe"]), [qT, Sb[0], Sb[1], Pm, av], [ob])
                if MLSTOP == 4:
                    continue
                obv = ob[:, 0:272].rearrange("p (h c) -> p h c", h=4)
                cd, fac, hd = B["cd"], B["fac"], B["hd"]
                kb.op("dve", lambda e: e.tensor_tensor(out=cd[:], in0=obv[:, :, 64], in1=cc[:], op=ALU.mult), reads=[ob, cc], writes=[cd])
                kb.op("act", lambda e: e.activation(out=cd[:], in_=cd[:], func=AF.Abs), reads=[cd], writes=[cd])
                kb.op("dve", lambda e: e.tensor_scalar_max(out=cd[:], in0=cd[:], scalar1=1.0), reads=[cd], writes=[cd])
                kb.op("dve", lambda e: e.reciprocal(out=cd[:], in_=cd[:]), reads=[cd], writes=[cd])
                kb.op("dve", lambda e: e.tensor_tensor(out=fac[:], in0=cd[:], in1=cc[:], op=ALU.mult), reads=[cd, cc], writes=[fac])
                kb.op("dve", lambda e: e.tensor_tensor(out=hd[:], in0=obv[:, :, 0:64], in1=fac[:, :, None].to_broadcast([128, 4, 64]), op=ALU.mult), reads=[ob, fac], writes=[hd])
                if d == 0:
                    kb.dma("pool", hf_d.ap()[t * 128:(t + 1) * 128, :], hd[:].rearrange("p h d -> p (h d)"), reads=[hd], writes=[hf_d])
                else:
                    hf, sq, s4, sg, mo = B["hf"], B["sq"], B["s4"], B["sg"], B["mo"]
                    kb.op("pool", lambda e: e.tensor_tensor(out=hd[:], in0=hd[:], in1=hf[:], op=ALU.add), reads=[hd, hf], writes=[hd])
                    kb.op("pool", lambda e: e.tensor_tensor(out=sq[:], in0=hd[:], in1=hd[:], op=ALU.mult), reads=[hd], writes=[sq])
                    kb.op("dve", lambda e: e.tensor_reduce(out=s4[:], in_=sq[:], axis=AX.X, op=ALU.add), reads=[sq], writes=[s4])
                    kb.op("act", lambda e: e.activation(out=s4[:], in_=s4[:], func=AF.Sqrt, bias=epst[:], scale=1.0 / 64.0), reads=[s4, epst], writes=[s4])
                    kb.op("dve", lambda e: e.reciprocal(out=s4[:], in_=s4[:]), reads=[s4], writes=[s4])
                    kb.op("dve", lambda e: e.tensor_tensor(out=hd[:], in0=hd[:], in1=s4[:, :, None].to_broadcast([128, 4, 64]), op=ALU.mult), reads=[hd, s4], writes=[hd])
                    kb.op("act", lambda e: e.activation(out=sg[:], in_=zm[:, 768:1024], func=AF.Sigmoid), reads=[zm], writes=[sg])
                    kb.op("pool", lambda e: e.tensor_tensor(out=sg[:], in0=sg[:], in1=gml[:], op=ALU.mult), reads=[sg, gml], writes=[sg])
                    kb.op("pool", lambda e: e.tensor_tensor(out=mo[:], in0=sg[:], in1=hd[:].rearrange("p h d -> p (h d)"), op=ALU.mult), reads=[sg, hd], writes=[mo])
                    kb.dma("pool", mix_d.ap()[t * 128:(t + 1) * 128, 768:1024], mo[:], reads=[mo], writes=[mix_d])
                if MLSTOP == 5:
                    continue
                gp = pb[4]
                for c in range(2):
                    for par in range(2):
                        hh = 2 * c + par
                        kb.op("pe", lambda e, c=c, hh=hh: e.matmul(gp[:, hh * 68:hh * 68 + 65], lhsT=kbm[:, c * 128:(c + 1) * 128], rhs=av[:, hh, 0:65], start=True, stop=True), reads=[kbm, av], writes=[gp], inc=(hh == 3))
                kb._commit((kb.csem["pe"], kb.ccnt["pe"]), [kbm, av], [gp])
                gpv = gp[:, 0:272].rearrange("p (c r k) -> p c r k", c=2, r=2)
                eblv = ebl[:].rearrange("p (c r) -> p c r", c=2)
                tmpS = B["tmpS"]
                for par in range(2):
                    ps_ = slice(par * 64, (par + 1) * 64)
                    kb.op("dve", lambda e, par=par, ps_=ps_: e.tensor_tensor(out=tmpS[ps_, :, :], in0=St[ps_, :, 0:65], in1=gpv[ps_, :, par, 0:65], op=ALU.add), reads=[St, gp], writes=[(tmpS, par)])
                    kb.op("dve", lambda e, par=par, ps_=ps_: e.tensor_tensor(out=St[ps_, :, 0:65], in0=tmpS[ps_, :, :], in1=eblv[ps_, :, par][:, :, None].to_broadcast([64, 2, 65]), op=ALU.mult), reads=[(tmpS, par), ebl], writes=[St])
                kb.op("dve", lambda e: e.tensor_copy(out=Sb[0][0:64, :, :], in_=St[0:64, :, :]), reads=[St], writes=[Sb[0]])
                kb.op("dve", lambda e: e.tensor_copy(out=Sb[1][64:128, :, :], in_=St[64:128, :, :]), reads=[St], writes=[Sb[1]])
        S.close()

        S = Scope(kb)
        wo_sb = S.sb("wo_sb", [128, 8, D], BF16)
        for kc in range(8):
            kb.dma("pool", wo_sb[:, kc, :], w['w_out'].ap()[kc * 128:(kc + 1) * 128, :], writes=[wo_sb])
        rw_sb = S.sb("rw_sb", [128, 8, NE], BF16)
        kb.dma("pool", rw_sb[:], w['router_w'].ap().rearrange("(kc p) e -> p kc e", p=128), writes=[rw_sb])
        FB = []
        for i in range(2):
            FB.append({n: S.sb("%s_%d" % (n, i), shp, dt) for n, shp, dt in [
                ("mt", [128, D], BF16), ("mT", [128, 8, 128], BF16), ("xt", [128, D], F32), ("x1", [128, D], F32), ("tmp", [128, D], F32),
                ("junk", [128, D], F32), ("xn", [128, D], BF16), ("ss", [128, 1], F32), ("hT", [128, 8, 128], BF16),
                ("lg", [128, NE], F32), ("mx", [128, 1], F32), ("sm", [128, 1], F32), ("afT", [NE, 128], F32)]})
        for t in range(NTILE if 'F' in phases else 0):
            B = FB[t % 2]
            j = 1 if t < 2 else 0
            mt, mT, xt, x1, tmp = B["mt"], B["mT"], B["xt"], B["x1"], B["tmp"]
            kb.dma("sp", mt[:], mix_d.ap()[t * 128:(t + 1) * 128, :], reads=[mix_d], writes=[mt])
            kb.dma("sp", xt[:], cur.ap()[t * 128:(t + 1) * 128, :], reads=[cur], writes=[xt])
            tp = pb[7]
            tpb = tp[:].bitcast(BF16)
            for kc in range(8):
                kb.op("pe", lambda e, kc=kc: e.transpose(out=tpb[:, kc * 128:(kc + 1) * 128], in_=mt[:, kc * 128:(kc + 1) * 128], identity=identb), reads=[mt, cstb], writes=[tp], inc=(kc == 7))
            kb.op("act", lambda e: e.copy(out=mT[:].rearrange("p k t -> p (k t)"), in_=tpb[:, :]), reads=[tp], writes=[mT])
            for n in range(2):
                bank = pb[1 + n]
                for kc in range(8):
                    kb.op("pe", lambda e, kc=kc, n=n: e.matmul(bank[:, :], lhsT=mT[:, kc, :], rhs=wo_sb[:, kc, n * 512:(n + 1) * 512], start=(kc == 0), stop=(kc == 7)), reads=[mT, wo_sb], writes=[bank], inc=(kc == 7))
                kb.op("dve", lambda e, n=n: e.tensor_tensor(out=tmp[:, n * 512:(n + 1) * 512], in0=bank[:, :], in1=gbc[:, 0, j, n * 512:(n + 1) * 512], op=ALU.mult), reads=[bank, gbc], writes=[tmp])
            kb.op("pool", lambda e: e.tensor_tensor(out=x1[:], in0=tmp[:], in1=xt[:], op=ALU.add), reads=[tmp, xt], writes=[x1])
            kb.dma("pool", xnext.ap()[t * 128:(t + 1) * 128, :], x1[:], reads=[x1], writes=[xnext])
            norm_T(B, x1, 1, j, B["hT"], "f")
            kb.dma("pool", xn2_d.ap()[t * 128:(t + 1) * 128, :], B["xn"][:], reads=[B["xn"]], writes=[xn2_d])
            lb = pb[3]
            for kc in range(8):
                kb.op("pe", lambda e, kc=kc: e.matmul(lb[:, 0:NE], lhsT=B["hT"][:, kc, :], rhs=rw_sb[:, kc, :], start=(kc == 0), stop=(kc == 7)), reads=[B["hT"], rw_sb], writes=[lb], inc=(kc == 7))
            lg, mx, sm, afT = B["lg"], B["mx"], B["sm"], B["afT"]
            kb.op("dve", lambda e: e.tensor_reduce(out=mx[:], in_=lb[:, 0:NE], axis=AX.X, op=ALU.max), reads=[lb], writes=[mx])
            kb.op("dve", lambda e: e.tensor_scalar(out=mx[:], in0=mx[:], scalar1=-1.0, scalar2=None, op0=ALU.mult), reads=[mx], writes=[mx])
            kb.op("act", lambda e: e.activation(out=lg[:], in_=lb[:, 0:NE], func=AF.Exp, bias=mx[:], scale=1.0, accum_out=sm[:]), reads=[lb, mx], writes=[lg, sm])
            kb.op("dve", lambda e: e.reciprocal(out=sm[:], in_=sm[:]), reads=[sm], writes=[sm])
            kb.op("dve", lambda e: e.tensor_scalar(out=lg[:], in0=lg[:], scalar1=sm[:], scalar2=None, op0=ALU.mult), reads=[lg, sm], writes=[lg])
            tb = pb[4]
            kb.op("pe", lambda e: e.transpose(out=tb[0:NE, 0:128], in_=lg[:], identity=identf), reads=[lg, cst], writes=[tb])
            kb.op("act", lambda e: e.copy(out=afT[:], in_=tb[0:NE, 0:128]), reads=[tb], writes=[afT])
            kb.dma("pool", affT_d.ap()[:, t * 128:(t + 1) * 128], afT[:], reads=[afT], writes=[affT_d])
        S.close()
        if l == 0:
            for nm_, src_, shp_, dt_ in [("x1", xnext, [NTOK, D], F32), ("affT", affT_d, [NE, NTOK], F32), ("mixall", mix_d, [NTOK, D], BF16)]:
                d_ = dbgout(nm_, shp_, dt_)
                if d_ is not None:
                    kb.dma("sp", d_.ap(), src_.ap(), reads=[src_], writes=[d_])

        if with_moe and 'G' in phases:
            S = Scope(kb)
            sets = [(CTX, SEQ, CAP, 0)] + ([(0, CTX, CAPC, 1)] if l == 0 else [])
            SD = []
            for si, (r0, ntok, cap, j) in enumerate(sets):
                J = ntok // 128
                V = S.sb("V%d" % si, [128, NE, J], F32)
                kb.dma("sp", V[:], affT_d.ap()[:, r0:r0 + ntok].rearrange("e (p j) -> p e j", p=128), reads=[affT_d], writes=[V], allow_slow_non_contiguous=True)
                M = S.sb("M%d" % si, [128, NE, J], F32)
                lo, hi, mid, cnt, d1, pred = [S.sb("%s%d" % (n, si), [128, NE], F32) for n in ("lo", "hi", "mid", "cnt", "d1", "pred")]
                kb.op("pool", lambda e: e.memset(lo[:], 0.0), writes=[lo])
                kb.op("pool", lambda e: e.memset(hi[:], 1.0), writes=[hi])
                for it in range(30):
                    kb.op("dve", lambda e: e.tensor_tensor(out=mid[:], in0=lo[:], in1=hi[:], op=ALU.add), reads=[lo, hi], writes=[mid])
                    kb.op("dve", lambda e: e.tensor_scalar(out=mid[:], in0=mid[:], scalar1=0.5, scalar2=None, op0=ALU.mult), reads=[mid], writes=[mid])
                    kb.op("dve", lambda e: e.tensor_tensor(out=M[:], in0=V[:], in1=mid[:, :, None].to_broadcast([128, NE, J]), op=ALU.is_ge), reads=[V, mid], writes=[M])
                    kb.op("dve", lambda e: e.tensor_reduce(out=cnt[:], in_=M[:], axis=AX.X, op=ALU.add), reads=[M], writes=[cnt])
                    tb = pb[7]
                    kb.op("pe", lambda e: e.matmul(tb[:, 0:NE], lhsT=onesf, rhs=cnt[:], start=True, stop=True), reads=[cst, cnt], writes=[tb])
                    kb.op("dve", lambda e: e.tensor_single_scalar(out=pred[:], in_=tb[:, 0:NE], scalar=float(cap), op=ALU.is_ge), reads=[tb], writes=[pred])
                    kb.op("dve", lambda e: e.tensor_tensor(out=d1[:], in0=mid[:], in1=lo[:], op=ALU.subtract), reads=[mid, lo], writes=[d1])
                    kb.op("dve", lambda e: e.tensor_tensor(out=d1[:], in0=d1[:], in1=pred[:], op=ALU.mult), reads=[d1, pred], writes=[d1])
                    kb.op("dve", lambda e: e.tensor_tensor(out=lo[:], in0=lo[:], in1=d1[:], op=ALU.add), reads=[lo, d1], writes=[lo])
                    kb.op("dve", lambda e: e.tensor_tensor(out=d1[:], in0=hi[:], in1=mid[:], op=ALU.subtract), reads=[hi, mid], writes=[d1])
                    kb.op("dve", lambda e: e.tensor_tensor(out=d1[:], in0=d1[:], in1=pred[:], op=ALU.mult), reads=[d1, pred], writes=[d1])
                    kb.op("dve", lambda e: e.tensor_tensor(out=hi[:], in0=mid[:], in1=d1[:], op=ALU.add), reads=[mid, d1], writes=[hi])
                kb.op("dve", lambda e: e.tensor_tensor(out=M[:], in0=V[:], in1=lo[:, :, None].to_broadcast([128, NE, J]), op=ALU.is_ge), reads=[V, lo], writes=[M])
                SD.append((V, M, J))
            w1_sb = S.sb("w1_sb", [128, 8, FF], BF16)
            w3_sb = S.sb("w3_sb", [128, 8, FF], BF16)
            w2_sb = S.sb("w2_sb", [128, 16, D], BF16)
            iot = S.sb("iot", [128, 2048], F32)
            kb.dma("sp", iot[:], iota.ap(), writes=[iot])
            rt, offt, endt = [S.sb(n, [128, 1], F32) for n in ("rt", "offt", "endt")]
            MT = S.sb("MT", [128, 128], F32)
            R = S.sb("R", [128, 258], F32)
            OH = S.sb("OH", [128, 2048], F32)
            XB = []
            for i in range(2):
                XB.append({n: S.sb("%s_%d" % (n, i), shp, dt) for n, shp, dt in [
                    ("g", [128, 258], F32), ("rel", [128, 1], F32), ("jk", [128, 128], F32), ("jidx", [128, 1], F32), ("gate", [128, 1], F32),
                    ("idxf", [128, 1], F32), ("idxi", [128, 1], I32), ("xs", [128, D], BF16), ("xsT", [128, 8, 128], BF16),
                    ("sa", [128, 512], F32), ("hid", [128, FF], BF16), ("hidT", [128, 16, 128], BF16), ("yo", [128, D], F32), ("xr", [128, D], F32)]})
            bi = 0
            for e_ in range(NE):
                for kc in range(8):
                    kb.dma("pool", w1_sb[:, kc, :], w['exp_w1'].ap()[e_, kc * 128:(kc + 1) * 128, :], writes=[w1_sb])
                    kb.dma("pool", w3_sb[:, kc, :], w['exp_w3'].ap()[e_, kc * 128:(kc + 1) * 128, :], writes=[w3_sb])
                for kc in range(16):
                    kb.dma("pool", w2_sb[:, kc, :], w['exp_w2'].ap()[e_, kc * 128:(kc + 1) * 128, :], writes=[w2_sb])
                for si, (r0, ntok, cap, j) in enumerate(sets):
                    V, M, J = SD[si]
                    Me = M[:, e_, :]
                    kb.op("dve", lambda e: e.tensor_reduce(out=rt[:], in_=Me, axis=AX.X, op=ALU.add), reads=[M], writes=[rt])
                    tb = pb[7]
                    kb.op("pe", lambda e: e.matmul(tb[:, 0:1], lhsT=triS, rhs=rt[:], start=True, stop=True), reads=[cst, rt], writes=[tb])
                    kb.op("dve", lambda e: e.tensor_copy(out=offt[:], in_=tb[:, 0:1]), reads=[tb], writes=[offt])
                    kb.op("dve", lambda e: e.tensor_tensor(out=endt[:], in0=offt[:], in1=rt[:], op=ALU.add), reads=[offt, rt], writes=[endt])
                    kb.op("pe", lambda e: e.transpose(out=tb[0:J, 128:256], in_=Me, identity=identf), reads=[M, cst], writes=[tb])
                    kb.op("dve", lambda e: e.tensor_copy(out=MT[0:J, :], in_=tb[0:J, 128:256]), reads=[tb], writes=[MT])
                    kb.op("pe", lambda e: e.matmul(tb[:, 256:256 + J], lhsT=MT[0:J, :], rhs=cst[0:J, 1, 0:J], start=True, stop=True), reads=[MT, cst], writes=[tb])
                    kb.op("dve", lambda e: e.tensor_copy(out=R[:, 0:J], in_=tb[:, 256:256 + J]), reads=[tb], writes=[R])
                    kb.op("dve", lambda e: e.tensor_copy(out=R[:, J:J + 1], in_=offt[:]), reads=[offt], writes=[R])
                    kb.op("dve", lambda e: e.tensor_copy(out=R[:, J + 1:J + 2], in_=cst[:, 9, 0:1]), reads=[cst], writes=[R])
                    kb.op("dve", lambda e: e.tensor_copy(out=R[:, J + 2:2 * J + 2], in_=V[:, e_, :]), reads=[V], writes=[R])
                    kb.op("dve", lambda e: e.tensor_scalar(out=OH[:, 0:cap], in0=iot[:, 0:cap], scalar1=offt[:], scalar2=None, op0=ALU.is_ge), reads=[iot, offt], writes=[OH])
                    kb.op("dve", lambda e: e.scalar_tensor_tensor(out=OH[:, 0:cap], in0=iot[:, 0:cap], scalar=endt[:], in1=OH[:, 0:cap], op0=ALU.is_lt, op1=ALU.mult), reads=[iot, endt, OH], writes=[OH])
                    nblk = max(1, cap // 128)
                    nb = min(128, cap)
                    for b in range(nblk):
                        B = XB[bi % 2]
                        bi += 1
                        g, rel, jk, jidx, gate, idxf, idxi = B["g"], B["rel"], B["jk"], B["jidx"], B["gate"], B["idxf"], B["idxi"]
                        gb_ = pb[6]
                        kb.op("pe", lambda e: e.matmul(gb_[0:nb, 0:2 * J + 2], lhsT=OH[:, b * 128:b * 128 + nb], rhs=R[:, 0:2 * J + 2], start=True, stop=True), reads=[OH, R], writes=[gb_])
                        kb.op("dve", lambda e: e.tensor_copy(out=g[0:nb, 0:2 * J + 2], in_=gb_[0:nb, 0:2 * J + 2]), reads=[gb_], writes=[g])
                        kb.op("dve", lambda e: e.tensor_scalar(out=rel[0:nb, :], in0=g[0:nb, J:J + 1], scalar1=-1.0, scalar2=cst[0:nb, 9, b:b + 1], op0=ALU.mult, op1=ALU.add), reads=[g, cst], writes=[rel])
                        kb.op("dve", lambda e: e.tensor_scalar(out=jk[0:nb, 0:J], in0=g[0:nb, 0:J], scalar1=rel[0:nb, :], scalar2=None, op0=ALU.is_le), reads=[g, rel], writes=[jk])
                        kb.op("dve", lambda e: e.tensor_reduce(out=jidx[0:nb, :], in_=jk[0:nb, 0:J], axis=AX.X, op=ALU.add), reads=[jk], writes=[jidx])
                        kb.op("dve", lambda e: e.scalar_tensor_tensor(out=jk[0:nb, 0:J], in0=iot[0:nb, 0:J], scalar=jidx[0:nb, :], in1=g[0:nb, J + 2:2 * J + 2], op0=ALU.is_equal, op1=ALU.mult), reads=[iot, jidx, g], writes=[jk])
                        kb.op("dve", lambda e: e.tensor_reduce(out=gate[0:nb, :], in_=jk[0:nb, 0:J], axis=AX.X, op=ALU.add), reads=[jk], writes=[gate])
                        kb.op("dve", lambda e: e.tensor_scalar(out=idxf[0:nb, :], in0=g[0:nb, J + 1:J + 2], scalar1=float(J), scalar2=jidx[0:nb, :], op0=ALU.mult, op1=ALU.add), reads=[g, jidx], writes=[idxf])
                        kb.op("dve", lambda e: e.tensor_scalar(out=idxi[0:nb, :], in0=idxf[0:nb, :], scalar1=float(r0), scalar2=None, op0=ALU.add), reads=[idxf], writes=[idxi])
                        xs, xsT, sa, hid, hidT, yo = B["xs"], B["xsT"], B["sa"], B["hid"], B["hidT"], B["yo"]
                        kb.gather(xs[0:nb, :], xn2_d.ap()[:, :], idxi[0:nb, :], NTOK - 1, reads=[xn2_d, idxi], writes=[xs])
                        tp = pb[0]
                        tpb = tp[:].bitcast(BF16)
                        for kc in range(8):
                            kb.op("pe", lambda e, kc=kc: e.transpose(out=tpb[:, kc * 128:kc * 128 + nb], in_=xs[0:nb, kc * 128:(kc + 1) * 128], identity=cstb[0:nb, 0, 0:nb]), reads=[xs, cstb], writes=[tp], inc=(kc == 7))
                        for kc in range(8):
                            kb.op("dve" if kc % 2 == 0 else "pool", lambda e, kc=kc: e.tensor_scalar(out=xsT[:, kc, 0:nb], in0=tpb[:, kc * 128:kc * 128 + nb], scalar1=AB[:, 1, 0, kc, j:j + 1], scalar2=AB[:, 1, 1, kc, j:j + 1], op0=ALU.mult, op1=ALU.add), reads=[tp, AB], writes=[xsT]) if kc % 2 == 0 else \
                                kb.op("act", lambda e, kc=kc: e.activation(out=xsT[:, kc, 0:nb], in_=tpb[:, kc * 128:kc * 128 + nb], func=AF.Identity, scale=AB[:, 1, 0, kc, j:j + 1], bias=AB[:, 1, 1, kc, j:j + 1]), reads=[tp, AB], writes=[xsT])
                        for n in range(4):
                            ba, bb = pb[1 + n % 2], pb[3 + n % 2]
                            for kc in range(8):
                                kb.op("pe", lambda e, kc=kc, n=n: e.matmul(ba[0:nb, :], lhsT=xsT[:, kc, 0:nb], rhs=w1_sb[:, kc, n * 512:(n + 1) * 512], start=(kc == 0), stop=(kc == 7)), reads=[xsT, w1_sb], writes=[ba], inc=(kc == 7))
                            for kc in range(8):
                                kb.op("pe", lambda e, kc=kc, n=n: e.matmul(bb[0:nb, :], lhsT=xsT[:, kc, 0:nb], rhs=w3_sb[:, kc, n * 512:(n + 1) * 512], start=(kc == 0), stop=(kc == 7)), reads=[xsT, w3_sb], writes=[bb], inc=(kc == 7))
                            kb.op("act", lambda e: e.activation(out=sa[0:nb, :], in_=ba[0:nb, :], func=AF.Silu), reads=[ba], writes=[sa])
                            kb.op("dve", lambda e, n=n: e.tensor_tensor(out=hid[0:nb, n * 512:(n + 1) * 512], in0=sa[0:nb, :], in1=bb[0:nb, :], op=ALU.mult), reads=[sa, bb], writes=[hid])
                        for half in range(2):
                            for k8 in range(8):
                                kc = half * 8 + k8
                                kb.op("pe", lambda e, kc=kc, k8=k8: e.transpose(out=tpb[:, k8 * 128:k8 * 128 + nb], in_=hid[0:nb, kc * 128:(kc + 1) * 128], identity=cstb[0:nb, 0, 0:nb]), reads=[hid, cstb], writes=[tp], inc=(k8 == 7))
                            kb.op("act", lambda e, half=half: e.copy(out=hidT[:, half * 8:(half + 1) * 8, 0:nb], in_=tpb[:, :].rearrange("p (k t) -> p k t", k=8)[:, :, 0:nb]), reads=[tp], writes=[hidT])
                        for n in range(2):
                            by = pb[5]
                            for kc in range(16):
                                kb.op("pe", lambda e, kc=kc, n=n: e.matmul(by[0:nb, :], lhsT=hidT[:, kc, 0:nb], rhs=w2_sb[:, kc, n * 512:(n + 1) * 512], start=(kc == 0), stop=(kc == 15)), reads=[hidT, w2_sb], writes=[by], inc=(kc == 15))
                            kb.op("dve", lambda e, n=n: e.scalar_tensor_tensor(out=yo[0:nb, n * 512:(n + 1) * 512], in0=by[0:nb, :], scalar=gate[0:nb, :], in1=gbc[0:nb, 1, j, n * 512:(n + 1) * 512], op0=ALU.mult, op1=ALU.mult), reads=[by, gate, gbc], writes=[yo])
                        xr = B["xr"]
                        kb.gather(xr[0:nb, :], xnext.ap()[:, :], idxi[0:nb, :], NTOK - 1, reads=[xnext, idxi], writes=[xr])
                        kb.op("pool", lambda e: e.tensor_tensor(out=xr[0:nb, :], in0=xr[0:nb, :], in1=yo[0:nb, :], op=ALU.add), reads=[xr, yo], writes=[xr])
                        kb.scatter(xnext.ap()[:, :], xr[0:nb, :], idxi[0:nb, :], NTOK - 1, reads=[xr, idxi], writes=[xnext])
            S.close()
        cur = xnext
    S = Scope(kb)
    fg = S.sb("fg", [128, D], F32)
    kb.dma("sp", fg[:], fng.ap().partition_broadcast(128), writes=[fg])
    OB = []
    for i in range(2):
        OB.append({n: S.sb("%s_%d" % (n, i), shp, dt) for n, shp, dt in [("xt", [128, D], F32), ("junk", [128, D], F32), ("ss", [128, 1], F32), ("o", [128, D], F32)]})
    for t in range(2, NTILE if 'N' in phases else 2):
        B = OB[t % 2]
        xt, junk, ss, o = B["xt"], B["junk"], B["ss"], B["o"]
        kb.dma("sp", xt[:], cur.ap()[t * 128:(t + 1) * 128, :], reads=[cur], writes=[xt])
        kb.op("act", lambda e: e.activation(out=junk[:], in_=xt[:], func=AF.Square, scale=1.0 / 32.0, accum_out=ss[:]), reads=[xt], writes=[junk, ss])
        kb.op("act", lambda e: e.activation(out=ss[:], in_=ss[:], func=AF.Sqrt, bias=epst[:], scale=1.0), reads=[ss, epst], writes=[ss])
        kb.op("dve", lambda e: e.reciprocal(out=ss[:], in_=ss[:]), reads=[ss], writes=[ss])
        kb.op("dve", lambda e: e.scalar_tensor_tensor(out=o[:], in0=xt[:], scalar=ss[:], in1=fg[:], op0=ALU.mult, op1=ALU.mult), reads=[xt, ss, fg], writes=[o])
        kb.dma("pool", out.ap()[(t - 2) * 128:(t - 1) * 128, :], o[:], reads=[o], writes=[out])
    S.close()
    kb.finish()
    return kb, dbg_t


def host_inputs(inp):
    f = np.float32
    m = {}
    m["xin"] = np.ascontiguousarray(np.concatenate([inp['ctx'][0], inp['x'][0]], axis=0), dtype=f)
    cv = np.stack([inp['c'][0], inp['c_ctx']], axis=-1)
    m["cvec"] = np.ascontiguousarray(cv.reshape(8, 128, 2).transpose(1, 0, 2), dtype=f)
    pos = np.arange(SEQ)
    row, col = pos // 64, pos % 64
    inv = (10000.0 ** (-np.arange(0, 32, 2, dtype=np.float32) / 32)).astype(f)
    def ang(p):
        a = p.astype(f)[:, None] * inv[None, :]
        return np.concatenate([a, a], axis=-1)
    an = np.concatenate([ang(row), ang(col)], axis=-1)
    cos, sin = np.cos(an).astype(f), np.sin(an).astype(f)
    sgn = np.tile(np.concatenate([-np.ones(16, f), np.ones(16, f)]), 2)
    rp = np.zeros((NTOK, 128), f)
    rp[:CTX, 0:64] = 1.0
    rp[CTX:, 0:64] = cos
    rp[CTX:, 64:128] = sin * sgn[None, :]
    m["rope"] = rp
    c = np.zeros((128, 10, 128), f)
    ii = np.arange(128)
    c[:, 0, :] = np.eye(128)
    c[:, 1, :] = (ii[:, None] <= ii[None, :])
    c[:, 2, :] = (ii[:, None] >= ii[None, :])
    c[:, 3, :] = 1.0
    c[:, 4, :] = (ii[:, None] < ii[None, :])
    c[:, 5, :] = (ii[:, None] == ii[None, :] - 1)
    c[:, 6, :] = (ii[:, None] == ii[None, :] + 1)
    c[127, 7, 0] = 1.0
    c[0, 8, 127] = 1.0
    for b in range(16):
        c[:, 9, b] = ii + 128 * b
    m["consts"] = c
    m["iota"] = np.ascontiguousarray(np.broadcast_to(np.arange(2048, dtype=f)[None, :], (128, 2048)))
    for l in range(2):
        for n in ['ada_w', 'norm1_g', 'w_in', 'conv_w', 'cmlp_ws', 'cmlp_bs', 'w_out', 'norm2_g', 'router_w',
                  'exp_w1', 'exp_w3', 'exp_w2']:
            m["%s%d" % (n, l)] = np.ascontiguousarray(inp[n][l], dtype=f)
        for n in ['ada_b', 'cmlp_norm_g', 'q_norm_g', 'k_norm_g', 'ml_norm_g']:
            m["%s%d" % (n, l)] = np.ascontiguousarray(inp[n][l][None, :], dtype=f)
        m["ml_gb%d" % l] = np.ascontiguousarray(np.concatenate([inp['ml_igate_b'][l], inp['ml_fgate_b'][l]])[None, :], dtype=f)
    m["final_norm_g"] = np.ascontiguousarray(inp['final_norm_g'][None, :], dtype=f)
    return m


def kernel(**inp):
    inp = {k: np.asarray(v) for k, v in inp.items()}
    kb, _ = build()
    m = host_inputs(inp)
    res = run_bass_kernel_spmd(kb.nc, [m], core_ids=[0])
    o = np.asarray(res.results[0]["out"], dtype=np.float32)
    return o.reshape(1, SEQ, D)
```
